# Optimizing a Trainium2 kernel written in Bass

```python
import math
import jax, jax.numpy as jnp
from jax import lax
import numpy as np

D_MODEL = 2048
BATCH = 2
SEQ = 16384
DEPTH = 2

N_HEADS = 16
HEAD_DIM = D_MODEL // N_HEADS // 2
V_DIM = 2 * HEAD_DIM
ROT_DIM = HEAD_DIM // 4
ROPE_THETA = 500000.0
Q_BLOCK = 128
CONV_WIDTH = 31
CONV_DIM = D_MODEL
D_FF = 5632
N_EXPERTS = 8
TOP_K = 2
D_FF_EXPERT = 7168
EXPERT_BLOCK = 256
ALPHA = (2.0 * DEPTH) ** 0.25
BETA = (8.0 * DEPTH) ** -0.25
LN_EPS = 1e-5

kernel_name = "hybrid_diffattn_conformer_moe_deepnorm"


def layer_norm(x, g, b):
    xf = x.astype(jnp.float32)
    mu = jnp.mean(xf, axis=-1, keepdims=True)
    var = jnp.mean(jnp.square(xf - mu), axis=-1, keepdims=True)
    y = (xf - mu) * lax.rsqrt(var + LN_EPS) * g.astype(jnp.float32) + b.astype(jnp.float32)
    return y.astype(x.dtype)


def rms_norm(x, g):
    xf = x.astype(jnp.float32)
    y = xf * lax.rsqrt(jnp.mean(jnp.square(xf), axis=-1, keepdims=True) + LN_EPS)
    return (y * g.astype(jnp.float32)).astype(x.dtype)


def rope_tables(positions):
    inv_freq = ROPE_THETA ** (-jnp.arange(0, ROT_DIM, 2, dtype=jnp.float32) / ROT_DIM)
    ang = positions.astype(jnp.float32)[..., None] * inv_freq
    return jnp.cos(ang)[:, :, None, :], jnp.sin(ang)[:, :, None, :]


def apply_partial_rope(t, cos, sin):
    cos = cos.astype(t.dtype)
    sin = sin.astype(t.dtype)
    half = ROT_DIM // 2
    r1, r2, rest = t[..., :half], t[..., half:ROT_DIM], t[..., ROT_DIM:]
    return jnp.concatenate([r1 * cos - r2 * sin, r2 * cos + r1 * sin, rest], axis=-1)


def diff_attention(x, cos, sin, w_qkv, lq1, lk1, lq2, lk2, subln_g, w_o, lambda_init):
    B, S, _ = x.shape
    nq = S // Q_BLOCK
    qk_w = N_HEADS * 2 * HEAD_DIM
    qkv = x @ w_qkv
    q, k, v = jnp.split(qkv, [qk_w, 2 * qk_w], axis=-1)
    q = apply_partial_rope(q.reshape(B, S, 2 * N_HEADS, HEAD_DIM), cos, sin) * (HEAD_DIM ** -0.5)
    k = apply_partial_rope(k.reshape(B, S, 2 * N_HEADS, HEAD_DIM), cos, sin)
    q_blocks = q.reshape(B, nq, Q_BLOCK, N_HEADS, 2, HEAD_DIM).transpose(1, 0, 3, 4, 2, 5)
    k = k.reshape(B, S, N_HEADS, 2, HEAD_DIM).transpose(0, 2, 3, 1, 4)
    v = v.reshape(B, S, N_HEADS, V_DIM).transpose(0, 2, 1, 3)
    lam = (jnp.exp(jnp.sum(lq1.astype(jnp.float32) * lk1.astype(jnp.float32)))
           - jnp.exp(jnp.sum(lq2.astype(jnp.float32) * lk2.astype(jnp.float32)))
           + lambda_init)
    key_idx = jnp.arange(S)
    starts = jnp.arange(nq) * Q_BLOCK

    def block(args):
        qb, start = args
        s = jnp.einsum('bhcqd,bhckd->bhcqk', qb, k).astype(jnp.float32)
        q_idx = start + jnp.arange(Q_BLOCK)
        causal = key_idx[None, :] <= q_idx[:, None]
        p = jax.nn.softmax(jnp.where(causal, s, -jnp.inf), axis=-1)
        a = p[:, :, 0] - lam * p[:, :, 1]
        o = jnp.einsum('bhqk,bhkv->bhqv', a.astype(v.dtype), v)
        return rms_norm(o, subln_g) * (1.0 - lambda_init)

    o = lax.map(block, (q_blocks, starts))
    o = o.transpose(1, 0, 3, 2, 4).reshape(B, S, N_HEADS * V_DIM)
    return o @ w_o


def conformer_conv(x, w_in, b_in, w_dw, b_dw, ln_g, ln_b, w_out, b_out):
    h = x @ w_in + b_in
    a, g = jnp.split(h, 2, axis=-1)
    h = a * jax.nn.sigmoid(g)
    h = lax.conv_general_dilated(
        h, w_dw[:, None, :], window_strides=(1,), padding=[(CONV_WIDTH - 1, 0)],
        dimension_numbers=('NWC', 'WIO', 'NWC'), feature_group_count=CONV_DIM) + b_dw
    h = jax.nn.silu(layer_norm(h, ln_g, ln_b))
    return h @ w_out + b_out


def swiglu(h, w_gu, w_down):
    g, u = jnp.split(h @ w_gu, 2, axis=-1)
    return (jax.nn.silu(g) * u) @ w_down


def moe_swiglu(x, w_router, w_gu, w_down):
    B, S, D = x.shape
    T = B * S
    A = T * TOP_K
    nb = -(-A // EXPERT_BLOCK) + N_EXPERTS
    P = nb * EXPERT_BLOCK
    xf = x.reshape(T, D)
    logits = (xf @ w_router).astype(jnp.float32)
    top_v, top_i = lax.top_k(logits, TOP_K)
    gates = jax.nn.softmax(top_v, axis=-1).astype(x.dtype)
    e_flat = top_i.reshape(A)
    g_flat = gates.reshape(A)
    tok_flat = jnp.arange(A, dtype=jnp.int32) // TOP_K
    counts = jnp.zeros((N_EXPERTS,), jnp.int32).at[e_flat].add(1)
    starts = jnp.cumsum(counts) - counts
    padded = (counts + EXPERT_BLOCK - 1) // EXPERT_BLOCK * EXPERT_BLOCK
    pends = jnp.cumsum(padded)
    pstarts = pends - padded
    order = jnp.argsort(e_flat)
    e_sorted = e_flat[order]
    dest = pstarts[e_sorted] + (jnp.arange(A, dtype=jnp.int32) - starts[e_sorted])
    slot_tok = jnp.zeros((P,), jnp.int32).at[dest].set(tok_flat[order])
    slot_gate = jnp.zeros((P,), x.dtype).at[dest].set(g_flat[order])
    block_start = jnp.arange(nb, dtype=jnp.int32) * EXPERT_BLOCK
    block_exp = jnp.clip(jnp.searchsorted(pends, block_start, side='right'), 0, N_EXPERTS - 1)

    def expert_block(args):
        tok, gate, ex = args
        return swiglu(xf[tok], w_gu[ex], w_down[ex]) * gate[:, None]

    ys = lax.map(expert_block, (slot_tok.reshape(nb, EXPERT_BLOCK),
                                slot_gate.reshape(nb, EXPERT_BLOCK), block_exp))
    y = jax.ops.segment_sum(ys.reshape(P, D), slot_tok, num_segments=T)
    return y.reshape(B, S, D)


def setup_inputs(seed: int = 0) -> dict:
    key = jax.random.key(seed)
    ks = iter(jax.random.split(key, 40))
    f32 = jnp.float32

    def nrm(shape, scale):
        return jax.random.normal(next(ks), shape, f32) * scale

    def gain(n):
        return 1.0 + nrm((n,), 0.02)

    D = D_MODEL
    qkv_w = 2 * (N_HEADS * 2 * HEAD_DIM) + N_HEADS * V_DIM
    inp = {}
    inp['x'] = nrm((BATCH, SEQ, D), 1.0)
    inp['positions'] = (jnp.arange(SEQ, dtype=jnp.int32)[None, :]
                        + jax.random.randint(next(ks), (BATCH, 1), 0, 4096, jnp.int32))
    inp['l0_w_qkv'] = nrm((D, qkv_w), D ** -0.5)
    inp['l0_lambda_q1'] = nrm((HEAD_DIM,), 0.1)
    inp['l0_lambda_k1'] = nrm((HEAD_DIM,), 0.1)
    inp['l0_lambda_q2'] = nrm((HEAD_DIM,), 0.1)
    inp['l0_lambda_k2'] = nrm((HEAD_DIM,), 0.1)
    inp['l0_subln_g'] = gain(V_DIM)
    inp['l0_w_o'] = nrm((N_HEADS * V_DIM, D), (N_HEADS * V_DIM) ** -0.5 * BETA)
    inp['l0_ln1_g'] = gain(D)
    inp['l0_ln1_b'] = nrm((D,), 0.02)
    inp['l0_ffn_w_gu'] = nrm((D, 2 * D_FF), D ** -0.5)
    inp['l0_ffn_w_down'] = nrm((D_FF, D), D_FF ** -0.5 * BETA)
    inp['l0_ln2_g'] = gain(D)
    inp['l0_ln2_b'] = nrm((D,), 0.02)
    inp['l1_conv_w_in'] = nrm((D, 2 * CONV_DIM), D ** -0.5)
    inp['l1_conv_b_in'] = nrm((2 * CONV_DIM,), 0.02)
    inp['l1_conv_w_dw'] = nrm((CONV_WIDTH, CONV_DIM), CONV_WIDTH ** -0.5)
    inp['l1_conv_b_dw'] = nrm((CONV_DIM,), 0.02)
    inp['l1_conv_ln_g'] = gain(CONV_DIM)
    inp['l1_conv_ln_b'] = nrm((CONV_DIM,), 0.02)
    inp['l1_conv_w_out'] = nrm((CONV_DIM, D), CONV_DIM ** -0.5 * BETA)
    inp['l1_conv_b_out'] = nrm((D,), 0.02)
    inp['l1_ln1_g'] = gain(D)
    inp['l1_ln1_b'] = nrm((D,), 0.02)
    inp['l1_moe_w_router'] = nrm((D, N_EXPERTS), D ** -0.5)
    inp['l1_moe_w_gu'] = nrm((N_EXPERTS, D, 2 * D_FF_EXPERT), D ** -0.5)
    inp['l1_moe_w_down'] = nrm((N_EXPERTS, D_FF_EXPERT, D), D_FF_EXPERT ** -0.5 * BETA)
    inp['l1_ln2_g'] = gain(D)
    inp['l1_ln2_b'] = nrm((D,), 0.02)
    return inp


def reference(x, positions,
              l0_w_qkv, l0_lambda_q1, l0_lambda_k1, l0_lambda_q2, l0_lambda_k2, l0_subln_g, l0_w_o,
              l0_ln1_g, l0_ln1_b, l0_ffn_w_gu, l0_ffn_w_down, l0_ln2_g, l0_ln2_b,
              l1_conv_w_in, l1_conv_b_in, l1_conv_w_dw, l1_conv_b_dw, l1_conv_ln_g, l1_conv_ln_b,
              l1_conv_w_out, l1_conv_b_out, l1_ln1_g, l1_ln1_b,
              l1_moe_w_router, l1_moe_w_gu, l1_moe_w_down, l1_ln2_g, l1_ln2_b):
    cos, sin = rope_tables(positions)
    layers = [
        dict(mix=lambda h: diff_attention(h, cos, sin, l0_w_qkv, l0_lambda_q1, l0_lambda_k1,
                                          l0_lambda_q2, l0_lambda_k2, l0_subln_g, l0_w_o,
                                          0.8 - 0.6 * math.exp(-0.3 * 0)),
             ln1=(l0_ln1_g, l0_ln1_b),
             ffn=lambda h: swiglu(h, l0_ffn_w_gu, l0_ffn_w_down),
             ln2=(l0_ln2_g, l0_ln2_b)),
        dict(mix=lambda h: conformer_conv(h, l1_conv_w_in, l1_conv_b_in, l1_conv_w_dw, l1_conv_b_dw,
                                          l1_conv_ln_g, l1_conv_ln_b, l1_conv_w_out, l1_conv_b_out),
             ln1=(l1_ln1_g, l1_ln1_b),
             ffn=lambda h: moe_swiglu(h, l1_moe_w_router, l1_moe_w_gu, l1_moe_w_down),
             ln2=(l1_ln2_g, l1_ln2_b)),
    ]
    for i in range(DEPTH):
        layer = layers[i]
        x = layer_norm(ALPHA * x + layer['mix'](x), *layer['ln1'])
        x = layer_norm(ALPHA * x + layer['ffn'](x), *layer['ln2'])
    return x
```

```python
import contextlib
import math
import numpy as np
import concourse.bass as bass
import concourse.mybir as mybir
from concourse.bass_utils import run_bass_kernel_spmd

F32 = mybir.dt.float32
BF16 = mybir.dt.bfloat16
I32 = mybir.dt.int32
AF = mybir.ActivationFunctionType
ALU = mybir.AluOpType
AX = mybir.AxisListType

D_MODEL = 2048
N_HEADS = 16
HEAD_DIM = 64
V_DIM = 128
ROT_DIM = 16
ROPE_THETA = 500000.0
LN_EPS = 1e-5
LAMBDA_INIT = 0.8 - 0.6 * math.exp(-0.3 * 0)
NEG = -30000.0


class KB:
    def __init__(self, nc, es):
        self.nc = nc
        self.es = es
        self.eng = {"pe": nc.tensor, "act": nc.scalar, "dve": nc.vector,
                    "pool": nc.gpsimd, "sp": nc.sync}
        self.sem = {k: es.enter_context(nc.semaphore("done_" + k)) for k in self.eng}
        self.cnt = {k: 0 for k in self.eng}
        self.seen = {k: {} for k in self.eng}
        self.dsem_cnt = {}
        self.n_dsem = 0

    def wait(self, e, tok):
        if tok is None:
            return
        if isinstance(tok, list):
            for t in tok:
                self.wait(e, t)
            return
        kind, src, val = tok
        key = src if kind == "e" else id(src)
        if self.seen[e].get(key, 0) >= val:
            return
        semh = self.sem[src] if kind == "e" else src
        self.eng[e].wait_ge(semh, val)
        self.seen[e][key] = val

    def op(self, e, fn, deps=(), inc=True):
        for d in deps:
            self.wait(e, d)
        ins = fn(self.eng[e])
        if inc:
            self.cnt[e] += 1
            ins.then_inc(self.sem[e], 1)
            return ("e", e, self.cnt[e])
        return None

    def dsem(self, name):
        s = self.es.enter_context(self.nc.semaphore(name))
        self.dsem_cnt[id(s)] = 0
        self.dsems = getattr(self, "dsems", []) + [s]
        return s

    def drain_dmas(self, e="sp"):
        for s in getattr(self, "dsems", []):
            if self.dsem_cnt[id(s)]:
                self.wait(e, ("d", s, self.dsem_cnt[id(s)]))

    def dma(self, e, out, in_, sem, deps=()):
        for d in deps:
            self.wait(e, d)
        self.eng[e].dma_start(out=out, in_=in_).then_inc(sem, 16)
        self.dsem_cnt[id(sem)] += 16
        return ("d", sem, self.dsem_cnt[id(sem)])

    def last(self, e):
        return ("e", e, self.cnt[e]) if self.cnt[e] else None


def bcast_rows(ap_1d_handle, n, parts=128, offset=0):
    return bass.AP(ap_1d_handle, offset, [[0, parts], [1, n]])


def build_attn(S, stop=99):
    NT = S // 128
    NQC = S // 512
    nc = bass.Bass("TRN2", target_bir_lowering=False)
    x_h = nc.dram_tensor("x", [S, D_MODEL], F32, kind="ExternalInput")
    w_h = nc.dram_tensor("wqkv", [4, D_MODEL, 384], F32, kind="ExternalInput")
    pos_h = nc.dram_tensor("pos", [128, NT], I32, kind="ExternalInput")
    lam_h = nc.dram_tensor("lamv", [256], F32, kind="ExternalInput")
    g_h = nc.dram_tensor("subg", [128], F32, kind="ExternalInput")
    o_h = nc.dram_tensor("o", [S, 512], F32, kind="ExternalOutput")
    x = x_h.ap()
    w = w_h.ap()
    o = o_h.ap()

    es = contextlib.ExitStack()
    with es:
        kb = KB(nc, es)
        sb = lambda name, shape, dt: es.enter_context(nc.sbuf_tensor(name, shape, dt))
        QT = sb("QT", [128, S], BF16)
        KT = sb("KT", [128, S], BF16)
        VA = sb("VA", [128, NT, 129], BF16)
        cos4 = sb("cos4", [128, NT, 4, 8], F32)
        sin4 = sb("sin4", [128, NT, 4, 8], F32)
        ident = sb("ident", [128, 128], BF16)
        trim = sb("trim", [128, 128], BF16)
        onesf = sb("onesf", [128, 128], F32)
        zerosf = sb("zerosf", [128, 128], F32)
        rl = [sb(f"rl{i}", [128, 4], F32) for i in range(2)]
        ss = sb("ss", [128, 4], F32)
        lamt = sb("lamt", [128, 256], F32)
        lamp = sb("lamp", [128, 128], F32)
        lams = sb("lams", [128, 2], F32)
        neglam = sb("neglam", [128, 1], F32)
        g4 = sb("g4", [128, 4, 128], F32)
        posi = sb("posi", [128, NT], I32)
        posf = sb("posf", [128, NT], F32)
        mhalf = sb("mhalf", [128, 4], F32)
        negpi = sb("negpi", [128, 1], F32)
        tmp_es = contextlib.ExitStack()
        sbt = lambda name, shape, dt: tmp_es.enter_context(nc.sbuf_tensor(name, shape, dt))
        ang = sbt("ang", [128, NT, 4, 8], F32)

        s_set = kb.dsem("s_set")
        t_pos = kb.dma("sp", posi[:], pos_h.ap(), s_set)
        t_lam = kb.dma("sp", lamt[:], bcast_rows(lam_h, 256), s_set)
        for j in range(4):
            t_g = kb.dma("sp", g4[:, j, :], bcast_rows(g_h, 128), s_set)
        t_set = t_g

        t1 = kb.op("pool", lambda e: e.memset(onesf[:], 1.0))
        t0 = kb.op("pool", lambda e: e.memset(zerosf[:], 0.0))
        t_id = kb.op("pool", lambda e: e.affine_select(
            out=ident[:], in_=onesf[:], pattern=[[-1, 128]], compare_op=ALU.is_equal,
            fill=0.0, base=0, channel_multiplier=1), deps=[t1])
        t_tr = kb.op("pool", lambda e: e.affine_select(
            out=trim[:], in_=zerosf[:], pattern=[[1, 128]], compare_op=ALU.is_ge,
            fill=NEG, base=0, channel_multiplier=-1), deps=[t0])
        t_ones = kb.op("pool", lambda e: e.memset(VA[:, :, 128:129], 1.0))

        t = kb.op("dve", lambda e: e.tensor_tensor(out=lamp[:].rearrange("p (a b) -> p a b", a=2),
                                                   in0=lamt[:].rearrange("p (a b c) -> p a b c", a=2, b=2)[:, :, 0, :],
                                                   in1=lamt[:].rearrange("p (a b c) -> p a b c", a=2, b=2)[:, :, 1, :],
                                                   op=ALU.mult), deps=[t_set])
        t = kb.op("dve", lambda e: e.tensor_reduce(out=lams[:], in_=lamp[:].rearrange("p (a b) -> p a b", a=2),
                                                   axis=AX.X, op=ALU.add), deps=[t])
        t = kb.op("act", lambda e: e.activation(lams[:], lams[:], AF.Exp), deps=[t])
        t = kb.op("dve", lambda e: e.tensor_tensor(out=neglam[:], in0=lams[:, 1:2], in1=lams[:, 0:1],
                                                   op=ALU.subtract), deps=[t])
        t_lamr = kb.op("dve", lambda e: e.tensor_scalar(out=neglam[:], in0=neglam[:], scalar1=-LAMBDA_INIT,
                                                        scalar2=None, op0=ALU.add), deps=[t])
        t_g4 = kb.op("dve", lambda e: e.tensor_scalar(out=g4[:], in0=g4[:], scalar1=(1.0 - LAMBDA_INIT),
                                                      scalar2=None, op0=ALU.mult), deps=[t_set])

        t = kb.op("dve", lambda e: e.tensor_copy(out=posf[:], in_=posi[:]), deps=[t_set])
        tl = []
        for i in range(8):
            f_i = ROPE_THETA ** (-(2 * i) / ROT_DIM)
            for m in range(4):
                tl.append(kb.op("dve", lambda e: e.tensor_scalar(out=ang[:, :, m, i], in0=posf[:], scalar1=float(f_i),
                                                                 scalar2=None, op0=ALU.mult), deps=[t]))
        t_ang = tl[-1]
        angf = ang[:].rearrange("p t m i -> p (t m i)")
        twopi = 2.0 * math.pi
        NA = NT * 32
        rr_k = sbt("rr_k", [128, NA], F32)
        rr_i = sbt("rr_i", [128, NA], I32)
        rr_m = sbt("rr_m", [128, NA], F32)

        def sincos(dst, shift, tdep):
            dstf = dst[:].rearrange("p t m i -> p (t m i)")
            t = kb.op("dve", lambda e: e.tensor_scalar(out=dstf, in0=angf, scalar1=float(shift), scalar2=None,
                                                       op0=ALU.add), deps=[t_ang, tdep])
            t = kb.op("dve", lambda e: e.tensor_scalar(out=rr_k[:], in0=dstf, scalar1=1.0 / twopi, scalar2=None,
                                                       op0=ALU.mult), deps=[t])
            t = kb.op("dve", lambda e: e.tensor_copy(out=rr_i[:], in_=rr_k[:]), deps=[t])
            t = kb.op("dve", lambda e: e.tensor_copy(out=rr_k[:], in_=rr_i[:]), deps=[t])
            t = kb.op("dve", lambda e: e.scalar_tensor_tensor(out=dstf, in0=rr_k[:], scalar=-twopi, in1=dstf,
                                                              op0=ALU.mult, op1=ALU.add), deps=[t])
            t = kb.op("dve", lambda e: e.tensor_scalar(out=rr_m[:], in0=dstf, scalar1=math.pi, scalar2=-twopi,
                                                       op0=ALU.is_gt, op1=ALU.mult), deps=[t])
            t = kb.op("dve", lambda e: e.tensor_tensor(out=dstf, in0=dstf, in1=rr_m[:], op=ALU.add), deps=[t])
            t = kb.op("dve", lambda e: e.tensor_scalar(out=dstf, in0=dstf, scalar1=-3.1415925, scalar2=3.1415925,
                                                       op0=ALU.max, op1=ALU.min), deps=[t])
            return kb.op("act", lambda e: e.activation(dstf, dstf, AF.Sin), deps=[t])

        t_sin = sincos(sin4, 0.0, None)
        t_cos = sincos(cos4, 0.5 * math.pi, t_sin)
        t_mh = kb.op("pool", lambda e: e.memset(mhalf[:], -0.5))
        tmp_es.close()
        t_setup_dve = kb.last("dve")
        xb = [sb(f"xb{i}", [128, D_MODEL], BF16) for i in range(2)]
        xT = [sb(f"xT{i}", [128, 16, 128], BF16) for i in range(2)]
        wb = [sb(f"wb{i}", [128, 16, 384], BF16) for i in range(2)]
        qs = [sb(f"qs{i}", [128, 4, 64], F32) for i in range(2)]
        qkb = [sb(f"qkb{i}", [128, 4, 64], BF16) for i in range(2)]
        tm = [sb(f"tm{i}", [128, 4, 8], F32) for i in range(4)]
        PT = [sb(f"PT{i}", [128, 512], BF16) for i in range(4)]
        Om = [[sb(f"Om{p}{m}", [128, 4, 128], F32) for m in range(2)] for p in range(2)]
        oo = [sb(f"oo{p}", [128, 4, 128], F32) for p in range(2)]
        sq = sb("sq", [128, 4, 128], F32)
        t_setup_all = [t_id, t_tr, t_ones, t_lamr, t_g4, t_sin, t_cos]

        s_x = [kb.dsem(f"s_x{i}") for i in range(2)]
        s_w = [kb.dsem(f"s_w{i}") for i in range(2)]
        s_o = [kb.dsem(f"s_o{i}") for i in range(2)]

        t_w = [None, None]
        t_w[0] = kb.dma("pool", wb[0][:], w[0].rearrange("(c p) n -> p c n", p=128), s_w[0], deps=[t_setup_dve, t_sin, t_cos])

        t_attn_done = None
        t_oo_free = [None, None]
        t_out = [None, None]
        for hh in range(4):
            if stop <= 1 or (hh >= 1 and stop < 99):
                break
            wcur = wb[hh % 2]
            with contextlib.ExitStack() as ps:
                pT = [ps.enter_context(nc.psum_tensor(f"pT{i}_{hh}", [128, 8, 128], BF16)) for i in range(2)]
                pQ = [ps.enter_context(nc.psum_tensor(f"pQ{i}_{hh}", [128, 384], F32)) for i in range(2)]
                pR = [[ps.enter_context(nc.psum_tensor(f"pR{i}{z}_{hh}", [128, 128], BF16)) for z in range(2)] for i in range(2)]

                tk_x = [None] * NT
                tk_T = [None] * NT
                tk_xT = [None] * NT
                tk_M = [None] * NT
                tk_qs = [None] * NT
                tk_v = [None] * NT
                tk_rope = [None] * NT
                tk_R = [None] * NT
                tk_RT = [None] * NT

                def load_x(t):
                    deps = [t_attn_done] if t < 2 else [tk_T[t - 2]]
                    tk_x[t] = kb.dma("pool", xb[t % 2][:], x[t * 128:(t + 1) * 128, :], s_x[t % 2], deps=deps)

                def stage_T(t):
                    deps = [tk_x[t], t_id]
                    if t >= 1:
                        deps.append(tk_xT[t - 1])
                    elif t_attn_done is not None:
                        deps.append(t_attn_done)
                    for d in deps:
                        kb.wait("pe", d)
                    for c in range(16):
                        kb.op("pe", lambda e: e.transpose(pT[c // 8][:, c % 8, :], xb[t % 2][:, c * 128:(c + 1) * 128],
                                                          ident[:]), inc=(c == 15))
                    tk_T[t] = kb.last("pe")
                    deps = [tk_T[t]]
                    if t >= 2:
                        deps.append(tk_M[t - 2])
                    a = kb.op("act", lambda e: e.copy(xT[t % 2][:, 0:8, :], pT[0][:]), deps=deps)
                    b = kb.op("dve", lambda e: e.tensor_copy(out=xT[t % 2][:, 8:16, :], in_=pT[1][:]), deps=deps)
                    tk_xT[t] = [a, b]

                def stage_M(t):
                    deps = [tk_xT[t], t_w[hh % 2]]
                    if t >= 2:
                        deps += [tk_qs[t - 2], tk_v[t - 2]]
                    for d in deps:
                        kb.wait("pe", d)
                    for c in range(16):
                        kb.op("pe", lambda e: e.matmul(pQ[t % 2][:], lhsT=xT[t % 2][:, c, :], rhs=wcur[:, c, :],
                                                       start=(c == 0), stop=(c == 15)), inc=(c == 15))
                    tk_M[t] = kb.last("pe")
                    deps = [tk_M[t]]
                    if t >= 2:
                        deps.append(tk_rope[t - 2])
                    tk_qs[t] = kb.op("act", lambda e: e.copy(qs[t % 2][:].rearrange("p a b -> p (a b)"),
                                                             pQ[t % 2][:, 0:256]), deps=deps)
                    dv = [tk_M[t], t_ones]
                    if t == 0 and t_attn_done is not None:
                        dv.append(t_attn_done)
                    tk_v[t] = kb.op("act", lambda e: e.copy(VA[:, t, 0:128], pQ[t % 2][:, 256:384]), deps=dv)
                    q = qs[t % 2]
                    ob = qkb[t % 2]
                    c4 = cos4[:, t, :, :]
                    s4 = sin4[:, t, :, :]
                    d0 = [tk_qs[t], t_sin, t_cos]
                    if t >= 2:
                        d0.append(tk_R[t - 2])
                    if t >= 1:
                        d0.append(tk_rope[t - 1])
                    a1 = kb.op("dve", lambda e: e.tensor_tensor(out=tm[0][:], in0=q[:, :, 0:8], in1=c4, op=ALU.mult), deps=d0)
                    a2 = kb.op("dve", lambda e: e.tensor_tensor(out=tm[1][:], in0=q[:, :, 8:16], in1=s4, op=ALU.mult), deps=d0)
                    a3 = kb.op("dve", lambda e: e.tensor_tensor(out=tm[2][:], in0=q[:, :, 8:16], in1=c4, op=ALU.mult), deps=d0)
                    a4 = kb.op("dve", lambda e: e.tensor_tensor(out=tm[3][:], in0=q[:, :, 0:8], in1=s4, op=ALU.mult), deps=d0)
                    b1 = kb.op("dve", lambda e: e.tensor_tensor(out=ob[:, :, 0:8], in0=tm[0][:], in1=tm[1][:], op=ALU.subtract), deps=[a1, a2])
                    b2 = kb.op("dve", lambda e: e.tensor_tensor(out=ob[:, :, 8:16], in0=tm[2][:], in1=tm[3][:], op=ALU.add), deps=[a3, a4])
                    b3 = kb.op("dve", lambda e: e.tensor_copy(out=ob[:, :, 16:64], in_=q[:, :, 16:64]), deps=d0)
                    tk_rope[t] = [b1, b2, b3]

                def stage_R(t):
                    deps = [tk_rope[t]]
                    if t >= 2:
                        deps.append(tk_RT[t - 2])
                    for d in deps:
                        kb.wait("pe", d)
                    obf = qkb[t % 2][:].rearrange("p a b -> p (a b)")
                    kb.op("pe", lambda e: e.transpose(pR[t % 2][0][:], obf[:, 0:128], ident[:]), inc=False)
                    tk_R[t] = kb.op("pe", lambda e: e.transpose(pR[t % 2][1][:], obf[:, 128:256], ident[:]))
                    dd = [tk_R[t]]
                    if t == 0 and t_attn_done is not None:
                        dd.append(t_attn_done)
                    a = kb.op("act", lambda e: e.copy(QT[:, t * 128:(t + 1) * 128], pR[t % 2][0][:]), deps=dd)
                    b = kb.op("dve", lambda e: e.tensor_copy(out=KT[:, t * 128:(t + 1) * 128], in_=pR[t % 2][1][:]), deps=dd)
                    tk_RT[t] = [a, b]

                load_x(0)
                if NT > 1:
                    load_x(1)
                stage_T(0)
                for t in range(NT):
                    if t + 1 < NT:
                        stage_T(t + 1)
                    if t + 2 < NT:
                        load_x(t + 2)
                    stage_M(t)
                    if t >= 1:
                        stage_R(t - 1)
                stage_R(NT - 1)
                t_proj_done = [tk_RT[NT - 1], tk_v[NT - 1], tk_RT[NT - 2] if NT > 1 else None]
                if hh + 1 < 4:
                    t_w[(hh + 1) % 2] = kb.dma("pool", wb[(hh + 1) % 2][:],
                                               w[hh + 1].rearrange("(c p) n -> p c n", p=128), s_w[(hh + 1) % 2])

            if stop <= 2:
                break
            with contextlib.ExitStack() as ps:
                pS = [ps.enter_context(nc.psum_tensor(f"pS{i}_{hh}", [128, 512], F32)) for i in range(4)]
                pO = [[ps.enter_context(nc.psum_tensor(f"pO{s}{i}_{hh}", [128, 2, 129], F32)) for i in range(2)]
                      for s in range(2)]
                for d in t_proj_done:
                    kb.wait("pe", d)
                    kb.wait("act", d)
                    kb.wait("dve", d)
                unit = 0
                tk_ev_unit = {}
                for qc in range(NQC):
                    par = qc % 2
                    tk_Om = [None, None]
                    for m in range(2):
                        oset = unit % 2
                        nk = 4 * qc + 4
                        kq = slice(64 * m, 64 * m + 64)
                        tk_S = [None] * nk
                        tk_P = [None] * nk
                        tk_AV = [None] * nk

                        def issue_S(kt, tk_AV=tk_AV, tk_S=tk_S, kq=kq, qc=qc):
                            buf = kt % 4
                            if kt >= 4:
                                kb.wait("pe", tk_P[kt - 4])
                            r = kt - 4 * qc
                            ksl = slice(kt * 128, (kt + 1) * 128)
                            if r < 0:
                                tk_S[kt] = kb.op("pe", lambda e: e.matmul(
                                    pS[buf][:], lhsT=KT[kq, ksl], rhs=QT[kq, qc * 512:(qc + 1) * 512],
                                    start=True, stop=True))
                            else:
                                c0 = qc * 512 + r * 128
                                kb.op("pe", lambda e: e.matmul(
                                    pS[buf][:, r * 128:(r + 1) * 128], lhsT=KT[kq, ksl], rhs=QT[kq, c0:c0 + 128],
                                    start=True, stop=False), inc=False)
                                tk = kb.op("pe", lambda e: e.matmul(
                                    pS[buf][:, r * 128:(r + 1) * 128], lhsT=ident[:], rhs=trim[:],
                                    start=False, stop=True), inc=(r == 3))
                                if r < 3:
                                    tk = kb.op("pe", lambda e: e.matmul(
                                        pS[buf][:, (r + 1) * 128:512], lhsT=KT[kq, ksl],
                                        rhs=QT[kq, c0 + 128:(qc + 1) * 512], start=True, stop=True))
                                tk_S[kt] = tk

                        def issue_exp(kt, tk_S=tk_S, tk_P=tk_P, tk_AV=tk_AV, qc=qc):
                            buf = kt % 4
                            r = max(0, kt - 4 * qc)
                            deps = [tk_S[kt]]
                            if kt >= 4:
                                deps.append(tk_AV[kt - 4])
                            tk_P[kt] = kb.op("act", lambda e: e.activation(
                                PT[buf][:, r * 128:512], pS[buf][:, r * 128:512], AF.Exp, scale=HEAD_DIM ** -0.5),
                                deps=deps)

                        def issue_AV(kt, tk_P=tk_P, tk_AV=tk_AV, qc=qc, oset=oset, unit=unit):
                            buf = kt % 4
                            r = max(0, kt - 4 * qc)
                            kb.wait("pe", tk_P[kt])
                            if kt == 0 and unit >= 2:
                                kb.wait("pe", tk_ev_unit[unit - 2])
                            for j in range(r, 4):
                                last = (kt == 4 * qc + j)
                                tk = kb.op("pe", lambda e: e.matmul(
                                    pO[oset][j // 2][:, j % 2, :], lhsT=PT[buf][:, j * 128:(j + 1) * 128],
                                    rhs=VA[:, kt, :], start=(kt == 0 and j % 2 == 0), stop=last, skip_group_check=True), inc=(j == 3))
                            tk_AV[kt] = tk

                        issue_S(0)
                        if nk > 1:
                            issue_S(1)
                        for kt in range(nk):
                            issue_exp(kt)
                            issue_AV(kt)
                            if kt + 2 < nk:
                                issue_S(kt + 2)
                        d = [tk_AV[nk - 1]]
                        if t_oo_free[par] is not None and m == 0:
                            pass
                        rr = rl[m]
                        omt = Om[par][m]
                        ta = kb.op("dve", lambda e: e.reciprocal(out=rr[:, 0:2], in_=pO[oset][0][:, :, 128]), deps=d)
                        tb = kb.op("dve", lambda e: e.reciprocal(out=rr[:, 2:4], in_=pO[oset][1][:, :, 128]), deps=d)
                        te = []
                        for j in range(4):
                            te.append(kb.op("dve", lambda e: e.tensor_scalar(
                                out=omt[:, j, :], in0=pO[oset][j // 2][:, j % 2, 0:128], scalar1=rr[:, j:j + 1],
                                scalar2=None, op0=ALU.mult), deps=[ta, tb]))
                        tk_ev_unit[unit] = te
                        tk_Om[m] = te
                        unit += 1
                    ot = oo[par]
                    d = [tk_Om[0], tk_Om[1], t_lamr, t_g4]
                    if t_out[par] is not None:
                        d.append(t_out[par])
                    t = kb.op("dve", lambda e: e.scalar_tensor_tensor(
                        out=ot[:], in0=Om[par][1][:], scalar=neglam[:, 0:1], in1=Om[par][0][:],
                        op0=ALU.mult, op1=ALU.add), deps=d)
                    t = kb.op("dve", lambda e: e.tensor_tensor(out=sq[:], in0=ot[:], in1=ot[:], op=ALU.mult), deps=[t])
                    t = kb.op("dve", lambda e: e.tensor_reduce(out=ss[:], in_=sq[:], axis=AX.X, op=ALU.add), deps=[t])
                    t = kb.op("dve", lambda e: e.tensor_scalar(out=ss[:], in0=ss[:], scalar1=1.0 / V_DIM, scalar2=LN_EPS,
                                                               op0=ALU.mult, op1=ALU.add), deps=[t])
                    t = kb.op("pool", lambda e: e.tensor_tensor(out=ss[:], in0=ss[:], in1=mhalf[:], op=ALU.pow),
                              deps=[t, t_mh])
                    tj = []
                    for j in range(4):
                        tj.append(kb.op("dve", lambda e: e.tensor_scalar(
                            out=ot[:, j, :], in0=ot[:, j, :], scalar1=ss[:, j:j + 1], scalar2=None, op0=ALU.mult),
                            deps=[t]))
                    t = kb.op("dve", lambda e: e.tensor_tensor(out=ot[:], in0=ot[:], in1=g4[:], op=ALU.mult), deps=tj)
                    t_out[par] = kb.dma(
                        "sp", o[qc * 512:(qc + 1) * 512, hh * 128:(hh + 1) * 128].rearrange("(j p) v -> p j v", p=128),
                        ot[:], s_o[par], deps=[t])
                t_attn_done = [kb.last("pe"), kb.last("act"), kb.last("dve")]
        if stop < 99:
            for e_ in ('pe', 'act', 'dve', 'pool'):
                kb.wait('sp', kb.last(e_))
            kb.wait('sp', t_w[0])
            kb.wait('sp', ('d', s_x[0], kb.dsem_cnt[id(s_x[0])]))
            kb.wait('sp', ('d', s_x[1], kb.dsem_cnt[id(s_x[1])]))
            t_out[0] = kb.dma('sp', o[0:128, 0:32], cos4[:, 0, :, :].rearrange('p a b -> p (a b)'), s_o[0])
        for p in range(2):
            kb.wait("sp", t_out[p])
        kb.drain_dmas("sp")
    return nc


def _attn_inputs(inputs, c, S):
    b, g = c // 4, c % 4
    W = np.asarray(inputs["l0_w_qkv"])
    qk_w = N_HEADS * 2 * HEAD_DIM
    blocks = []
    for hh in range(4):
        h = 4 * g + hh
        blocks.append(np.concatenate([W[:, 128 * h:128 * h + 128],
                                      W[:, qk_w + 128 * h:qk_w + 128 * h + 128],
                                      W[:, 2 * qk_w + 128 * h:2 * qk_w + 128 * h + 128]], axis=1))
    wqkv = np.ascontiguousarray(np.stack(blocks, 0), dtype=np.float32)
    lamv = np.concatenate([np.asarray(inputs["l0_lambda_q1"]), np.asarray(inputs["l0_lambda_k1"]),
                           np.asarray(inputs["l0_lambda_q2"]), np.asarray(inputs["l0_lambda_k2"])]).astype(np.float32)
    return {
        "x": np.ascontiguousarray(np.asarray(inputs["x"])[b, :S], dtype=np.float32),
        "wqkv": wqkv,
        "pos": np.ascontiguousarray(np.asarray(inputs["positions"])[b, :S].reshape(S // 128, 128).T, dtype=np.int32),
        "lamv": lamv,
        "subg": np.asarray(inputs["l0_subln_g"], dtype=np.float32),
    }


D_FF = 5632
D_FF_E = 7168
N_EXP = 8
CONV_W = 31
ALPHA = (2.0 * 2) ** 0.25
TB = 512
NRING = 3
V_LN1G, V_LN1B, V_LN2G, V_LN2B, V_CLNG, V_CLNB, V_L1G, V_L1B, V_L2G, V_L2B, V_BOUT, V_BDW, V_BINA, V_BING, V_DW0 = range(15)
NV = V_DW0 + CONV_W


class Tracker:
    def __init__(self, kb):
        self.kb = kb
        self.w = {}
        self.r = {}

    def deps(self, reads, writes):
        d = []
        for k in reads:
            if k in self.w:
                d.append(self.w[k])
        for k in writes:
            if k in self.w:
                d.append(self.w[k])
            d.extend(self.r.get(k, {}).values())
        return d

    def commit(self, tok, reads, writes):
        if tok is None:
            return
        key = (tok[0], tok[1] if tok[0] == "e" else id(tok[1]))
        for k in reads:
            self.r.setdefault(k, {})[key] = tok
        for k in writes:
            self.w[k] = tok
            self.r[k] = {}

    def op(self, e, fn, reads=(), writes=()):
        tok = self.kb.op(e, fn, deps=self.deps(reads, writes))
        self.commit(tok, reads, writes)
        return tok

    def group(self, e, emit, reads=(), writes=()):
        kb = self.kb
        for d in self.deps(reads, writes):
            kb.wait(e, d)
        ins = emit(kb.eng[e])
        kb.cnt[e] += 1
        ins.then_inc(kb.sem[e], 1)
        tok = ("e", e, kb.cnt[e])
        self.commit(tok, reads, writes)
        return tok

    def dma(self, e, out, in_, sem, reads=(), writes=()):
        tok = self.kb.dma(e, out, in_, sem, deps=self.deps(reads, writes))
        self.commit(tok, reads, writes)
        return tok


def build_tail(NBLK, moe=True, debug=False, stop_stage=9):
    NTOK = 128 + NBLK * TB
    nc = bass.Bass("TRN2", target_bir_lowering=False)
    dt_in = lambda name, shape, dt=F32: nc.dram_tensor(name, shape, dt, kind="ExternalInput").ap()
    xin = dt_in("xin", [NTOK, D_MODEL])
    oin = dt_in("oin", [NTOK, D_MODEL])
    wo = dt_in("wo", [D_MODEL, D_MODEL])
    wgu = dt_in("wgu", [D_MODEL, 2 * D_FF])
    wdn = dt_in("wdn", [D_FF, D_MODEL])
    win = dt_in("win", [D_MODEL, 2 * D_MODEL])
    wout = dt_in("wout", [D_MODEL, D_MODEL])
    wr = dt_in("wr", [128, 16, N_EXP])
    mgu = dt_in("mgu", [N_EXP, D_MODEL, 2 * D_FF_E])
    mdn = dt_in("mdn", [N_EXP, D_FF_E, D_MODEL])
    vecs = dt_in("vecs", [128, 16, NV])
    flag = dt_in("flag", [128, 1])
    out = nc.dram_tensor("out", [NBLK * TB, D_MODEL], F32, kind="ExternalOutput").ap()

    es = contextlib.ExitStack()
    with es:
        kb = KB(nc, es)
        tr = Tracker(kb)
        sb = lambda name, shape, dt: es.enter_context(nc.sbuf_tensor(name, shape, dt))
        RT = sb("RT", [128, 16, TB], F32)
        AT = sb("AT", [128, 16, TB], BF16)
        BIG = sb("BIG", [128, 16896], F32)
        HT = BIG[:].bitcast(BF16)[:, 0:56 * TB].rearrange("p (c t) -> p c t", c=56) if hasattr(BIG[:], "bitcast") else None
        ring = [sb(f"ring{i}", [128, 16, 512], BF16) for i in range(NRING)]
        vT = sb("vT", [128, 16, NV], F32)
        wrs = sb("wrs", [128, 16, N_EXP], F32)
        flg = sb("flg", [128, 1], F32)
        identf = sb("identf", [128, 128], F32)
        onesD = sb("onesD", [128, 128], F32)
        ones1 = sb("ones1", [128, 128], F32)
        sel = sb("sel", [8, N_EXP, 128], F32)
        carry = sb("carry", [128, 16, 30], F32)
        tmpA = [sb(f"tmpA{i}", [128, TB], F32) for i in range(3)]
        tmpD = [sb(f"tmpD{i}", [128, TB], F32) for i in range(3)]
        mean_t = sb("mean_t", [128, TB], F32)
        rstd_t = sb("rstd_t", [128, TB], F32)
        GBt = sb("GBt", [128, TB], F32)
        gateT = sb("gateT", [8, TB], F32)
        lgT = sb("lgT", [8, TB], F32)
        sm = {n: sb("sm_" + n, [128, 8], F32) for n in ["lg", "eq1", "l2", "eq2", "G"]}
        sc = {n: sb("sc_" + n, [128, 1], F32) for n in ["m1", "m2", "d", "g1", "g2"]}
        P = [es.enter_context(nc.psum_tensor(f"P{i}", [128, 512], F32)) for i in range(8)]
        HC = BIG[:, 0:16 * (TB + 30)].rearrange("p (c t) -> p c t", c=16)
        CV = BIG[:, 8704:8704 + 16 * TB].rearrange("p (c t) -> p c t", c=16)
        XS = BIG[:, 0:4 * D_MODEL].rearrange("p (i f) -> p i f", i=4)

        s_set = kb.dsem("s_set")
        s_io = [kb.dsem(f"s_io{i}") for i in range(4)]
        s_ring = [kb.dsem(f"s_ring{i}") for i in range(NRING)]
        s_out = [kb.dsem(f"s_out{i}") for i in range(4)]

        tr.dma("sp", vT[:], vecs, s_set, writes=["vT"])
        tr.dma("sp", wrs[:], wr, kb.dsem("s_set1"), writes=["wrs"])
        tr.dma("sp", flg[:], flag, kb.dsem("s_set2"), writes=["flg"])
        tr.op("pool", lambda e: e.memset(ones1[:], 1.0), writes=["ones1"])
        tr.op("pool", lambda e: e.memset(onesD[:], 1.0 / D_MODEL), writes=["onesD"])
        tr.op("pool", lambda e: e.affine_select(out=identf[:], in_=ones1[:], pattern=[[-1, 128]],
                                                compare_op=ALU.is_equal, fill=0.0, base=0, channel_multiplier=1),
              reads=["ones1"], writes=["identf"])
        for e_ in range(N_EXP):
            tr.op("dve", lambda e: e.tensor_scalar(out=sel[0:8, e_, :], in0=ones1[0:8, :], scalar1=identf[0:8, e_:e_ + 1],
                                                   scalar2=None, op0=ALU.mult), reads=["ones1", "identf"], writes=["sel"])
        tr.op("pool", lambda e: e.memset(carry[:], 0.0), writes=["carry"])

        class WS:
            def __init__(self):
                self.loads = []
                self.emitted = 0

            def plan(self, src, k0, nkc, col0):
                self.loads.append((src, k0, nkc, col0))

            def emit_upto(self, i):
                while self.emitted <= min(i, len(self.loads) - 1):
                    L = self.emitted
                    src, k0, nkc, col0 = self.loads[L]
                    s = L % NRING
                    tr.dma("pool", ring[s][:, 0:nkc, :],
                           src[k0 * 128:(k0 + nkc) * 128, col0:col0 + 512].rearrange("(c p) n -> p c n", p=128),
                           s_ring[s], writes=[("ring", s)])
                    self.emitted += 1

            def get(self, i, oldest=None):
                self.emit_upto((i if oldest is None else oldest) + NRING - 1)
                return ring[i % NRING], ("ring", i % NRING)

        ws = WS()
        blocks = [(0, 128, True)] + [(128 + b * TB, TB, False) for b in range(NBLK)]

        def plan_up(src, ncols, col0=0):
            for g in range(ncols // 512):
                ws.plan(src, 0, 16, col0 + g * 512)

        def plan_gu(src, dff):
            for g in range(dff // 512):
                ws.plan(src, 0, 16, g * 512)
                ws.plan(src, 0, 16, dff + g * 512)

        def plan_down(src, nkc_total):
            for g in range(4):
                k0 = 0
                while k0 < nkc_total:
                    n = min(16, nkc_total - k0)
                    ws.plan(src, k0, n, g * 512)
                    k0 += n

        for (_, T, is_halo) in blocks:
            plan_up(wo, D_MODEL)
            plan_gu(wgu, D_FF)
            plan_down(wdn, D_FF // 128)
            plan_gu(win, D_MODEL)
            if is_halo:
                continue
            plan_up(wout, D_MODEL)
            if moe:
                for e_ in range(N_EXP):
                    plan_gu(mgu[e_], D_FF_E)
                    plan_down(mdn[e_], D_FF_E // 128)
        wi = [0]

        pb = [0]

        def next_bank():
            b = pb[0]
            pb[0] = (pb[0] + 1) % 8
            return b

        def mm_up(T, load_i, sub, bank, src_act, src_key, oldest=None):
            slot, skey = ws.get(load_i, oldest)

            def emit(e):
                for c in range(16):
                    ins = e.matmul(P[bank][:, 0:T], lhsT=slot[:, c, sub * 128:(sub + 1) * 128], rhs=src_act[:, c, 0:T],
                                   start=(c == 0), stop=(c == 15))
                return ins
            tr.group("pe", emit, reads=[skey] + [(src_key, c) for c in range(16)], writes=[("P", bank)])

        def linear_res(T, ncols_chunks, bias_idx=None):
            for g in range(ncols_chunks // 4):
                li = wi[0]
                wi[0] += 1
                for sub in range(4):
                    n = g * 4 + sub
                    bank = next_bank()
                    mm_up(T, li, sub, bank, AT, "AT")
                    if bias_idx is None:
                        tr.op("dve", lambda e: e.scalar_tensor_tensor(out=RT[:, n, 0:T], in0=RT[:, n, 0:T], scalar=ALPHA,
                                                                       in1=P[bank][:, 0:T], op0=ALU.mult, op1=ALU.add),
                              reads=[("P", bank), ("RT", n)], writes=[("RT", n)])
                    else:
                        tr.op("dve", lambda e: e.tensor_scalar(out=RT[:, n, 0:T], in0=RT[:, n, 0:T], scalar1=ALPHA,
                                                               scalar2=vT[:, n, bias_idx:bias_idx + 1], op0=ALU.mult,
                                                               op1=ALU.add), reads=[("RT", n), "vT"], writes=[("RT", n)])
                        tr.op("dve", lambda e: e.tensor_tensor(out=RT[:, n, 0:T], in0=RT[:, n, 0:T], in1=P[bank][:, 0:T],
                                                               op=ALU.add), reads=[("P", bank), ("RT", n)], writes=[("RT", n)])

        def ln_fm(T, src, skf, gi, bi, silu=False, write_rt=True):
            bm, bv = next_bank(), next_bank()
            for c in range(16):
                ta = tmpA[c % 3]
                tr.op("act", lambda e: e.activation(ta[:, 0:T], src[:, c, 0:T], AF.Square),
                      reads=skf(c), writes=[("tmpA", c % 3)])
                tr.group("pe", lambda e: e.matmul(P[bm][:, 0:T], lhsT=onesD[:], rhs=src[:, c, 0:T], start=(c == 0),
                                                  stop=(c == 15)), reads=skf(c) + ["onesD"], writes=[("P", bm)])
                tr.group("pe", lambda e: e.matmul(P[bv][:, 0:T], lhsT=onesD[:], rhs=ta[:, 0:T], start=(c == 0),
                                                  stop=(c == 15)), reads=[("tmpA", c % 3), "onesD"], writes=[("P", bv)])
            tr.op("dve", lambda e: e.tensor_copy(out=mean_t[:, 0:T], in_=P[bm][:, 0:T]), reads=[("P", bm)], writes=["mean"])
            tr.op("dve", lambda e: e.tensor_tensor(out=tmpD[0][:, 0:T], in0=mean_t[:, 0:T], in1=mean_t[:, 0:T], op=ALU.mult),
                  reads=["mean"], writes=[("tmpD", 0)])
            tr.op("dve", lambda e: e.tensor_tensor(out=tmpD[0][:, 0:T], in0=P[bv][:, 0:T], in1=tmpD[0][:, 0:T],
                                                   op=ALU.subtract), reads=[("P", bv), ("tmpD", 0)], writes=[("tmpD", 0)])
            tr.op("dve", lambda e: e.tensor_scalar(out=tmpD[0][:, 0:T], in0=tmpD[0][:, 0:T], scalar1=LN_EPS, scalar2=None,
                                                   op0=ALU.add), reads=[("tmpD", 0)], writes=[("tmpD", 0)])
            tr.op("act", lambda e: e.activation(tmpD[0][:, 0:T], tmpD[0][:, 0:T], AF.Sqrt), reads=[("tmpD", 0)],
                  writes=[("tmpD", 0)])
            tr.op("dve", lambda e: e.reciprocal(out=rstd_t[:, 0:T], in_=tmpD[0][:, 0:T]), reads=[("tmpD", 0)],
                  writes=["rstd"])
            for c in range(16):
                td = tmpD[1 + c % 2]
                tk = ("tmpD", 1 + c % 2)
                tr.op("dve", lambda e: e.tensor_tensor(out=td[:, 0:T], in0=src[:, c, 0:T], in1=mean_t[:, 0:T],
                                                       op=ALU.subtract), reads=skf(c) + ["mean"], writes=[tk])
                tr.op("dve", lambda e: e.tensor_tensor(out=td[:, 0:T], in0=td[:, 0:T], in1=rstd_t[:, 0:T], op=ALU.mult),
                      reads=[tk, "rstd"], writes=[tk])
                g_ap = vT[:, c, gi:gi + 1]
                b_ap = vT[:, c, bi:bi + 1]
                if write_rt:
                    tr.op("dve", lambda e: e.tensor_scalar(out=RT[:, c, 0:T], in0=td[:, 0:T], scalar1=g_ap, scalar2=b_ap,
                                                           op0=ALU.mult, op1=ALU.add), reads=[tk, "vT"], writes=[("RT", c)])
                tr.op("act", lambda e: e.activation(AT[:, c, 0:T], td[:, 0:T], AF.Silu if silu else AF.Identity,
                                                    bias=b_ap, scale=g_ap), reads=[tk, "vT"], writes=[("AT", c)])

        def ffn(T, dff, evac_down):
            nch = dff // 128
            for g in range(dff // 512):
                lg_i, lu_i = wi[0], wi[0] + 1
                wi[0] += 2
                for sub in range(4):
                    j = g * 4 + sub
                    bg, bu = next_bank(), next_bank()
                    mm_up(T, lg_i, sub, bg, AT, "AT")
                    mm_up(T, lu_i, sub, bu, AT, "AT", oldest=lg_i)
                    ta = tmpA[j % 3]
                    tr.op("act", lambda e: e.activation(ta[:, 0:T], P[bg][:, 0:T], AF.Silu), reads=[("P", bg)],
                          writes=[("tmpA", j % 3)])
                    tr.op("dve", lambda e: e.tensor_tensor(out=HT[:, j, 0:T], in0=P[bu][:, 0:T], in1=ta[:, 0:T], op=ALU.mult),
                          reads=[("P", bu), ("tmpA", j % 3)], writes=[("BIG", j)])
            for g in range(4):
                banks = [next_bank() for _ in range(4)]
                k0 = 0
                while k0 < nch:
                    n_k = min(16, nch - k0)
                    slot, skey = ws.get(wi[0])
                    wi[0] += 1

                    def emit(e, k0=k0, n_k=n_k, slot=slot):
                        for kc in range(n_k):
                            for sub in range(4):
                                ins = e.matmul(P[banks[sub]][:, 0:T], lhsT=slot[:, kc, sub * 128:(sub + 1) * 128],
                                               rhs=HT[:, k0 + kc, 0:T], start=(k0 + kc == 0), stop=(k0 + kc == nch - 1))
                        return ins
                    tr.group("pe", emit, reads=[skey] + [("BIG", k0 + kc) for kc in range(n_k)],
                             writes=[("P", b) for b in banks])
                    k0 += n_k
                for sub in range(4):
                    evac_down(g * 4 + sub, banks[sub])

        def evac_res(T):
            def f(n, bank):
                tr.op("dve", lambda e: e.scalar_tensor_tensor(out=RT[:, n, 0:T], in0=RT[:, n, 0:T], scalar=ALPHA,
                                                               in1=P[bank][:, 0:T], op0=ALU.mult, op1=ALU.add),
                      reads=[("P", bank), ("RT", n)], writes=[("RT", n)])
            return f

        def evac_gated(T):
            def f(n, bank):
                td = tmpD[n % 3]
                tr.op("dve", lambda e: e.tensor_tensor(out=td[:, 0:T], in0=P[bank][:, 0:T], in1=GBt[:, 0:T], op=ALU.mult),
                      reads=[("P", bank), "GBt"], writes=[("tmpD", n % 3)])
                tr.op("dve", lambda e: e.tensor_tensor(out=RT[:, n, 0:T], in0=RT[:, n, 0:T], in1=td[:, 0:T], op=ALU.add),
                      reads=[("tmpD", n % 3), ("RT", n)], writes=[("RT", n)])
            return f

        def bk(lo, hi):
            return [("BIG", j) for j in range(lo // 1024, (hi - 1) // 1024 + 1)]
        big_all = bk(0, 67584)
        hck = lambda c: bk(c * 2168, (c + 1) * 2168)
        cvk = lambda c: bk(34816 + c * 2048, 34816 + (c + 1) * 2048)
        xsk = lambda si: bk(si * 8192, (si + 1) * 8192)
        xsq = lambda si, q: bk(si * 8192 + q * 2048, si * 8192 + (q + 1) * 2048)
        rtk = lambda c: [("RT", c)]

        for bi_, (tok0, T, is_halo) in enumerate(blocks):
            nt = T // 128
            for which, srcd, dst, dkey in (("x", xin, RT, "RT"), ("o", oin, AT, "AT")):
                for i in range(nt):
                    si = i % 4
                    tr.dma("sp", XS[:, si, :], srcd[tok0 + i * 128: tok0 + (i + 1) * 128, :], s_io[si],
                           writes=xsk(si))
                    for q in range(4):
                        bank = next_bank()

                        def emit(e, q=q, bank=bank, si=si):
                            for z in range(4):
                                c = q * 4 + z
                                ins = e.transpose(P[bank][:, z * 128:(z + 1) * 128], XS[:, si, c * 128:(c + 1) * 128], identf[:])
                            return ins
                        tr.group("pe", emit, reads=xsk(si) + ["identf"], writes=[("P", bank)])
                        eng = "act" if q % 2 == 0 else "dve"
                        if eng == "act":
                            tr.op("act", lambda e: e.copy(dst[:, q * 4:(q + 1) * 4, i * 128:(i + 1) * 128],
                                                          P[bank][:].rearrange("p (z t) -> p z t", z=4)),
                                  reads=[("P", bank)], writes=[(dkey, q * 4 + z) for z in range(4)])
                        else:
                            tr.op("dve", lambda e: e.tensor_copy(out=dst[:, q * 4:(q + 1) * 4, i * 128:(i + 1) * 128],
                                                                 in_=P[bank][:].rearrange("p (z t) -> p z t", z=4)),
                                  reads=[("P", bank)], writes=[(dkey, q * 4 + z) for z in range(4)])
            linear_res(T, 16)
            ln_fm(T, RT, rtk, V_LN1G, V_LN1B)
            full = stop_stage >= 9 or is_halo
            if full or stop_stage >= 2:
                ffn(T, D_FF, evac_res(T))
                ln_fm(T, RT, rtk, V_LN2G, V_LN2B)
            else:
                wi[0] += 34
            if full:
                tr.op("dve", lambda e: e.tensor_copy(out=HC[:, :, 0:30], in_=carry[:]), reads=["carry"], writes=bk(0, 34816))
                for g in range(4):
                    la_i, lg_i = wi[0], wi[0] + 1
                    wi[0] += 2
                    for sub in range(4):
                        c = g * 4 + sub
                        ba, bg = next_bank(), next_bank()
                        mm_up(T, la_i, sub, ba, AT, "AT")
                        mm_up(T, lg_i, sub, bg, AT, "AT", oldest=la_i)
                        ta = tmpA[c % 3]
                        tr.op("act", lambda e: e.activation(ta[:, 0:T], P[bg][:, 0:T], AF.Sigmoid,
                                                            bias=vT[:, c, V_BING:V_BING + 1]),
                              reads=[("P", bg), "vT"], writes=[("tmpA", c % 3)])
                        tr.op("dve", lambda e: e.scalar_tensor_tensor(out=HC[:, c, 30:30 + T], in0=P[ba][:, 0:T],
                                                                       scalar=vT[:, c, V_BINA:V_BINA + 1], in1=ta[:, 0:T],
                                                                       op0=ALU.add, op1=ALU.mult),
                              reads=[("P", ba), ("tmpA", c % 3), "vT"], writes=hck(c))
                if is_halo:
                    tr.op("dve", lambda e: e.tensor_scalar(out=carry[:], in0=HC[:, :, T:T + 30], scalar1=flg[:, 0:1],
                                                           scalar2=None, op0=ALU.mult),
                          reads=bk(0, 34816) + ["flg"], writes=["carry"])
                    continue
                tr.op("dve", lambda e: e.tensor_copy(out=carry[:], in_=HC[:, :, T:T + 30]),
                      reads=bk(0, 34816), writes=["carry"])
                for c in range(16):
                    eng = "dve"
                    tr.op(eng, lambda e: e.tensor_scalar(out=CV[:, c, 0:T], in0=HC[:, c, 0:T],
                                                         scalar1=vT[:, c, V_DW0:V_DW0 + 1], scalar2=vT[:, c, V_BDW:V_BDW + 1],
                                                         op0=ALU.mult, op1=ALU.add),
                          reads=hck(c) + ["vT"], writes=cvk(c))
                    for j in range(1, CONV_W):
                        tr.op(eng, lambda e: e.scalar_tensor_tensor(out=CV[:, c, 0:T], in0=HC[:, c, j:j + T],
                                                                    scalar=vT[:, c, V_DW0 + j:V_DW0 + j + 1], in1=CV[:, c, 0:T],
                                                                    op0=ALU.mult, op1=ALU.add),
                              reads=hck(c) + cvk(c) + ["vT"], writes=cvk(c))
                ln_fm(T, CV, cvk, V_CLNG, V_CLNB, silu=True, write_rt=False)
                linear_res(T, 16, bias_idx=V_BOUT)
                ln_fm(T, RT, rtk, V_L1G, V_L1B)
                if moe:
                    bl = next_bank()
                    for c in range(16):
                        tr.group("pe", lambda e: e.matmul(P[bl][0:8, 0:T], lhsT=wrs[:, c, :], rhs=RT[:, c, 0:T],
                                                          start=(c == 0), stop=(c == 15)),
                                 reads=["wrs", ("RT", c)], writes=[("P", bl)])
                    tr.op("dve", lambda e: e.tensor_copy(out=lgT[0:8, 0:T], in_=P[bl][0:8, 0:T]), reads=[("P", bl)], writes=["lgT"])
                    for i in range(nt):
                        bt = next_bank()
                        tr.group("pe", lambda e: e.transpose(P[bt][:, 0:8], lgT[0:8, i * 128:(i + 1) * 128], identf[0:8, 0:8]),
                                 reads=["lgT", "identf"], writes=[("P", bt)])
                        S_ = sm
                        tr.op("dve", lambda e: e.tensor_copy(out=S_["lg"][:], in_=P[bt][:, 0:8]), reads=[("P", bt)], writes=["lg"])
                        tr.op("dve", lambda e: e.tensor_reduce(out=sc["m1"][:], in_=S_["lg"][:], axis=AX.X, op=ALU.max),
                              reads=["lg"], writes=["m1"])
                        tr.op("dve", lambda e: e.tensor_scalar(out=S_["eq1"][:], in0=S_["lg"][:], scalar1=sc["m1"][:, 0:1],
                                                               scalar2=None, op0=ALU.is_equal), reads=["lg", "m1"], writes=["eq1"])
                        tr.op("dve", lambda e: e.scalar_tensor_tensor(out=S_["l2"][:], in0=S_["eq1"][:], scalar=-1e30,
                                                                       in1=S_["lg"][:], op0=ALU.mult, op1=ALU.add),
                              reads=["eq1", "lg"], writes=["l2"])
                        tr.op("dve", lambda e: e.tensor_reduce(out=sc["m2"][:], in_=S_["l2"][:], axis=AX.X, op=ALU.max),
                              reads=["l2"], writes=["m2"])
                        tr.op("dve", lambda e: e.tensor_scalar(out=S_["eq2"][:], in0=S_["l2"][:], scalar1=sc["m2"][:, 0:1],
                                                               scalar2=None, op0=ALU.is_equal), reads=["l2", "m2"], writes=["eq2"])
                        tr.op("dve", lambda e: e.tensor_tensor(out=sc["d"][:], in0=sc["m1"][:], in1=sc["m2"][:], op=ALU.subtract),
                              reads=["m1", "m2"], writes=["d"])
                        tr.op("act", lambda e: e.activation(sc["g1"][:], sc["d"][:], AF.Sigmoid), reads=["d"], writes=["g1"])
                        tr.op("dve", lambda e: e.tensor_scalar(out=sc["g2"][:], in0=sc["g1"][:], scalar1=-1.0, scalar2=1.0,
                                                               op0=ALU.mult, op1=ALU.add), reads=["g1"], writes=["g2"])
                        tr.op("dve", lambda e: e.tensor_scalar(out=S_["G"][:], in0=S_["eq1"][:], scalar1=sc["g1"][:, 0:1],
                                                               scalar2=None, op0=ALU.mult), reads=["eq1", "g1"], writes=["G"])
                        tr.op("dve", lambda e: e.scalar_tensor_tensor(out=S_["G"][:], in0=S_["eq2"][:], scalar=sc["g2"][:, 0:1],
                                                                       in1=S_["G"][:], op0=ALU.mult, op1=ALU.add),
                              reads=["eq2", "g2", "G"], writes=["G"])
                        bt2 = next_bank()
                        tr.group("pe", lambda e: e.transpose(P[bt2][0:8, 0:128], S_["G"][:], identf[:]),
                                 reads=["G", "identf"], writes=[("P", bt2)])
                        tr.op("dve", lambda e: e.tensor_copy(out=gateT[0:8, i * 128:(i + 1) * 128], in_=P[bt2][0:8, 0:128]),
                              reads=[("P", bt2)], writes=["gateT"])
                    for c in range(16):
                        tr.op("dve", lambda e: e.tensor_scalar(out=RT[:, c, 0:T], in0=RT[:, c, 0:T], scalar1=ALPHA, scalar2=None,
                                                               op0=ALU.mult), reads=[("RT", c)], writes=[("RT", c)])
                    for e_ in range(N_EXP):
                        bgt = next_bank()
                        tr.group("pe", lambda e: e.matmul(P[bgt][:, 0:T], lhsT=sel[0:8, e_, :], rhs=gateT[0:8, 0:T],
                                                          start=True, stop=True), reads=["sel", "gateT"], writes=[("P", bgt)])
                        tr.op("dve", lambda e: e.tensor_copy(out=GBt[:, 0:T], in_=P[bgt][:, 0:T]), reads=[("P", bgt)],
                              writes=["GBt"])
                        ffn(T, D_FF_E, evac_gated(T))
                    ln_fm(T, RT, rtk, V_L2G, V_L2B)
            else:
                wi[0] += 8 + 4 + (N_EXP * 44 if moe else 0)
            for i in range(nt):
                si = i % 4
                for q in range(4):
                    bank = next_bank()

                    def emit(e, q=q, bank=bank):
                        for z in range(4):
                            c = q * 4 + z
                            ins = e.transpose(P[bank][:, z * 128:(z + 1) * 128], RT[:, c, i * 128:(i + 1) * 128], identf[:])
                        return ins
                    tr.group("pe", emit, reads=[("RT", q * 4 + z) for z in range(4)] + ["identf"], writes=[("P", bank)])
                    if q % 2 == 0:
                        tr.op("act", lambda e: e.copy(XS[:, si, q * 512:(q + 1) * 512], P[bank][:]), reads=[("P", bank)],
                              writes=xsq(si, q))
                    else:
                        tr.op("dve", lambda e: e.tensor_copy(out=XS[:, si, q * 512:(q + 1) * 512], in_=P[bank][:]),
                              reads=[("P", bank)], writes=xsq(si, q))
                r0 = tok0 - 128 + i * 128
                tr.dma("sp", out[r0:r0 + 128, :], XS[:, si, :], s_out[si],
                       reads=xsk(si))
        kb.drain_dmas("sp")
    return nc


def _pack_vecs(inp):
    rows = [inp["l0_ln1_g"], inp["l0_ln1_b"], inp["l0_ln2_g"], inp["l0_ln2_b"], inp["l1_conv_ln_g"], inp["l1_conv_ln_b"],
            inp["l1_ln1_g"], inp["l1_ln1_b"], inp["l1_ln2_g"], inp["l1_ln2_b"], inp["l1_conv_b_out"], inp["l1_conv_b_dw"],
            np.asarray(inp["l1_conv_b_in"])[:D_MODEL], np.asarray(inp["l1_conv_b_in"])[D_MODEL:]]
    rows = [np.asarray(r, dtype=np.float32) for r in rows] + [np.asarray(inp["l1_conv_w_dw"], dtype=np.float32)[j] for j in range(CONV_W)]
    v = np.stack(rows, 0)
    return np.ascontiguousarray(v.reshape(NV, 16, 128).transpose(2, 1, 0))


def _tail_inputs(inp, xin, oin, flag):
    f = lambda k: np.ascontiguousarray(np.asarray(inp[k]), dtype=np.float32)
    return {
        "xin": np.ascontiguousarray(xin, dtype=np.float32), "oin": np.ascontiguousarray(oin, dtype=np.float32),
        "wo": f("l0_w_o"), "wgu": f("l0_ffn_w_gu"), "wdn": f("l0_ffn_w_down"), "win": f("l1_conv_w_in"),
        "wout": f("l1_conv_w_out"),
        "wr": np.ascontiguousarray(f("l1_moe_w_router").reshape(16, 128, N_EXP).transpose(1, 0, 2)),
        "mgu": f("l1_moe_w_gu"), "mdn": f("l1_moe_w_down"), "vecs": _pack_vecs(inp),
        "flag": np.full((128, 1), flag, np.float32),
    }


SEQ = 16384
BATCH = 2
NBLK_CORE = 8


def kernel(**inputs):
    inp = {k: np.asarray(v) for k, v in inputs.items()}
    cores = list(range(8))
    nc_a = build_attn(SEQ)
    maps_a = [_attn_inputs(inp, c, SEQ) for c in cores]
    res_a = run_bass_kernel_spmd(nc_a, maps_a, core_ids=cores)
    o_full = np.empty((BATCH, SEQ, D_MODEL), np.float32)
    for c in cores:
        b, g = c // 4, c % 4
        o_full[b, :, 512 * g:512 * (g + 1)] = res_a.results[c]["o"]
    del res_a, maps_a
    nc_b = build_tail(NBLK_CORE, moe=True)
    x = inp["x"]
    maps_b = []
    for c in cores:
        b, j = c // 4, c % 4
        t0 = j * 4096
        if j == 0:
            xin = np.concatenate([x[b, 0:128], x[b, 0:4096]], 0)
            oin = np.concatenate([o_full[b, 0:128], o_full[b, 0:4096]], 0)
        else:
            xin = x[b, t0 - 128:t0 + 4096]
            oin = o_full[b, t0 - 128:t0 + 4096]
        maps_b.append(_tail_inputs(inp, xin, oin, 0.0 if j == 0 else 1.0))
    res_b = run_bass_kernel_spmd(nc_b, maps_b, core_ids=cores)
    out = np.empty((BATCH, SEQ, D_MODEL), np.float32)
    for c in cores:
        b, j = c // 4, c % 4
        out[b, j * 4096:(j + 1) * 4096] = res_b.results[c]["out"]
    return out
```
